# Optimizing a Trainium2 kernel written in Bass

```python
import math
import jax, jax.numpy as jnp
from jax import lax
import numpy as np

D_MODEL = 2048
BATCH = 1
SEQ = 8192
DEPTH = 4

N_A_LAYERS = DEPTH // 2
N_B_LAYERS = DEPTH - N_A_LAYERS

N_HEADS_A = 16
HEAD_DIM_A = D_MODEL // N_HEADS_A
MOBA_BLOCK = 256
MOBA_TOPK = 3
MOBA_QCHUNK = 64

N_HEADS_B = 16
Q_LORA = 768
KV_LORA = 512
QK_NOPE = 128
QK_ROPE = 64
V_DIM = 128
ROPE_THETA = 10000.0
MLA_QCHUNK = 128

D_FF = 5632
EPS = 1e-6

kernel_name = "yoco_moba_mla_macaron_alibi"


def rmsnorm(x, g):
    xf = x.astype(jnp.float32)
    y = xf * lax.rsqrt(jnp.mean(xf * xf, axis=-1, keepdims=True) + EPS)
    return (y * g.astype(jnp.float32)).astype(x.dtype)


def swiglu(xn, w_gu, w_d):
    gate, up = jnp.split(xn @ w_gu, 2, axis=-1)
    return (jax.nn.silu(gate) * up) @ w_d


def alibi_slopes(n):
    return jnp.asarray(np.array([2.0 ** (-8.0 * (i + 1) / n) for i in range(n)], dtype=np.float32))


def rope_tables(s):
    inv = 1.0 / (ROPE_THETA ** (jnp.arange(0, QK_ROPE, 2, dtype=jnp.float32) / QK_ROPE))
    ang = jnp.arange(s, dtype=jnp.float32)[:, None] * inv[None, :]
    return jnp.cos(ang), jnp.sin(ang)


def apply_rope(x, cos, sin):
    xf = x.astype(jnp.float32)
    x1, x2 = jnp.split(xf, 2, axis=-1)
    out = jnp.concatenate([x1 * cos - x2 * sin, x1 * sin + x2 * cos], axis=-1)
    return out.astype(x.dtype)


def moba_attention(xn, w_qkv, w_o, slopes):
    B, S, _ = xn.shape
    H, dh, BS, QC = N_HEADS_A, HEAD_DIM_A, MOBA_BLOCK, MOBA_QCHUNK
    nb = -(-S // BS)
    topk = min(MOBA_TOPK, nb)
    q, k, v = jnp.split(xn @ w_qkv, 3, axis=-1)

    def heads(t):
        return t.reshape(B, S, H, dh).transpose(0, 2, 1, 3)

    q, k, v = heads(q), heads(k), heads(v)
    pad = nb * BS - S
    k_p = jnp.pad(k, ((0, 0), (0, 0), (0, pad), (0, 0)))
    v_p = jnp.pad(v, ((0, 0), (0, 0), (0, pad), (0, 0)))
    kb = k_p.reshape(B, H, nb, BS, dh)
    vb = v_p.reshape(B, H, nb, BS, dh)
    k_mean = jnp.mean(kb.astype(jnp.float32), axis=3)
    n_chunks = S // QC
    q_chunks = q.reshape(B, H, n_chunks, QC, dh).transpose(2, 0, 1, 3, 4)
    m = slopes.reshape(1, H, 1, 1)
    scale = dh ** -0.5
    gather_blocks = jax.vmap(jax.vmap(lambda tab, idx: tab[idx]))
    blk_ids = jnp.arange(nb)

    def chunk(args):
        ci, qc = args
        t0 = ci * QC
        cur = t0 // BS
        t = t0 + jnp.arange(QC)
        gate = jnp.einsum('bhqd,bhnd->bhqn', qc.astype(jnp.float32), k_mean)
        gate = jnp.where(blk_ids < cur, gate, -jnp.inf)
        _, sel = lax.top_k(gate, topk)
        sel_ok = sel < cur
        k_sel = gather_blocks(kb, sel)
        v_sel = gather_blocks(vb, sel)
        s_sel = jnp.einsum('bhqd,bhqnkd->bhqnk', qc, k_sel).astype(jnp.float32) * scale
        key_pos = sel[..., None] * BS + jnp.arange(BS)
        dist = (t[:, None, None] - key_pos).astype(jnp.float32)
        s_sel = jnp.where(sel_ok[..., None], s_sel - m[..., None] * dist, -jnp.inf)
        s_sel = s_sel.reshape(B, H, QC, topk * BS)
        k_own = lax.dynamic_slice_in_dim(k_p, cur * BS, BS, axis=2)
        v_own = lax.dynamic_slice_in_dim(v_p, cur * BS, BS, axis=2)
        own_pos = cur * BS + jnp.arange(BS)
        dist_own = (t[:, None] - own_pos[None, :]).astype(jnp.float32)
        s_own = jnp.einsum('bhqd,bhkd->bhqk', qc, k_own).astype(jnp.float32) * scale - m * dist_own
        s_own = jnp.where(dist_own >= 0, s_own, -jnp.inf)
        p = jax.nn.softmax(jnp.concatenate([s_sel, s_own], axis=-1), axis=-1).astype(v.dtype)
        p_sel = p[..., :topk * BS].reshape(B, H, QC, topk, BS)
        p_own = p[..., topk * BS:]
        return (jnp.einsum('bhqnk,bhqnkd->bhqd', p_sel, v_sel)
                + jnp.einsum('bhqk,bhkd->bhqd', p_own, v_own))

    o = lax.map(chunk, (jnp.arange(n_chunks), q_chunks))
    o = o.transpose(1, 0, 3, 2, 4).reshape(B, S, H * dh)
    return o @ w_o


def mla_shared_kv(h, kv_norm, w_dkv, ckv_norm, w_ukv, cos, sin):
    B, S, _ = h.shape
    H = N_HEADS_B
    hn = rmsnorm(h, kv_norm)
    c_kv, k_rope = jnp.split(hn @ w_dkv, [KV_LORA], axis=-1)
    c_kv = rmsnorm(c_kv, ckv_norm)
    k_rope = apply_rope(k_rope, cos, sin)
    kv = (c_kv @ w_ukv).reshape(B, S, H, QK_NOPE + V_DIM)
    k_nope, v = jnp.split(kv, [QK_NOPE], axis=-1)
    k = jnp.concatenate([k_nope, jnp.broadcast_to(k_rope[:, :, None, :], (B, S, H, QK_ROPE))], axis=-1)
    return k.transpose(0, 2, 1, 3), v.transpose(0, 2, 1, 3)


def mla_attention(xn, w_dq, cq_norm, w_uq, w_o, k, v, cos, sin):
    B, S, _ = xn.shape
    H, QC = N_HEADS_B, MLA_QCHUNK
    cq = rmsnorm(xn @ w_dq, cq_norm)
    q = (cq @ w_uq).reshape(B, S, H, QK_NOPE + QK_ROPE)
    q_nope, q_rope = jnp.split(q, [QK_NOPE], axis=-1)
    q_rope = apply_rope(q_rope, cos[:, None, :], sin[:, None, :])
    q = jnp.concatenate([q_nope, q_rope], axis=-1).transpose(0, 2, 1, 3)
    n_chunks = S // QC
    q_chunks = q.reshape(B, H, n_chunks, QC, QK_NOPE + QK_ROPE).transpose(2, 0, 1, 3, 4)
    scale = (QK_NOPE + QK_ROPE) ** -0.5
    key_pos = jnp.arange(S)

    def chunk(args):
        ci, qc = args
        t = ci * QC + jnp.arange(QC)
        s = jnp.einsum('bhqd,bhkd->bhqk', qc, k).astype(jnp.float32) * scale
        s = jnp.where(key_pos[None, :] <= t[:, None], s, -jnp.inf)
        p = jax.nn.softmax(s, axis=-1).astype(v.dtype)
        return jnp.einsum('bhqk,bhkd->bhqd', p, v)

    o = lax.map(chunk, (jnp.arange(n_chunks), q_chunks))
    o = o.transpose(1, 0, 3, 2, 4).reshape(B, S, H * V_DIM)
    return o @ w_o


def setup_inputs(seed: int = 0) -> dict:
    key = jax.random.key(seed)
    ks = jax.random.split(key, 24)
    f32 = jnp.float32

    def w(k, shape, fan_in):
        return jax.random.normal(k, shape, f32) * (fan_in ** -0.5)

    def gain(k, shape):
        return 1.0 + 0.02 * jax.random.normal(k, shape, f32)

    D, F = D_MODEL, D_FF
    return {
        "x": jax.random.normal(ks[0], (BATCH, SEQ, D), f32),
        "ln_ffn1": gain(ks[1], (DEPTH, D)),
        "ffn1_wgu": w(ks[2], (DEPTH, D, 2 * F), D),
        "ffn1_wd": w(ks[3], (DEPTH, F, D), F),
        "ln_mix": gain(ks[4], (DEPTH, D)),
        "ln_ffn2": gain(ks[5], (DEPTH, D)),
        "ffn2_wgu": w(ks[6], (DEPTH, D, 2 * F), D),
        "ffn2_wd": w(ks[7], (DEPTH, F, D), F),
        "moba_wqkv": w(ks[8], (N_A_LAYERS, D, 3 * N_HEADS_A * HEAD_DIM_A), D),
        "moba_wo": w(ks[9], (N_A_LAYERS, N_HEADS_A * HEAD_DIM_A, D), N_HEADS_A * HEAD_DIM_A),
        "kv_norm": gain(ks[10], (D,)),
        "mla_wdkv": w(ks[11], (D, KV_LORA + QK_ROPE), D),
        "ckv_norm": gain(ks[12], (KV_LORA,)),
        "mla_wukv": w(ks[13], (KV_LORA, N_HEADS_B * (QK_NOPE + V_DIM)), KV_LORA),
        "mla_wdq": w(ks[14], (N_B_LAYERS, D, Q_LORA), D),
        "cq_norm": gain(ks[15], (N_B_LAYERS, Q_LORA)),
        "mla_wuq": w(ks[16], (N_B_LAYERS, Q_LORA, N_HEADS_B * (QK_NOPE + QK_ROPE)), Q_LORA),
        "mla_wo": w(ks[17], (N_B_LAYERS, N_HEADS_B * V_DIM, D), N_HEADS_B * V_DIM),
        "final_norm": gain(ks[18], (D,)),
    }


def reference(x, ln_ffn1, ffn1_wgu, ffn1_wd, ln_mix, ln_ffn2, ffn2_wgu, ffn2_wd,
              moba_wqkv, moba_wo, kv_norm, mla_wdkv, ckv_norm, mla_wukv,
              mla_wdq, cq_norm, mla_wuq, mla_wo, final_norm):
    S = x.shape[1]
    slopes = alibi_slopes(N_HEADS_A)
    cos, sin = rope_tables(S)

    def macaron_layer(h, l, mixer):
        h = h + 0.5 * swiglu(rmsnorm(h, ln_ffn1[l]), ffn1_wgu[l], ffn1_wd[l])
        h = h + mixer(rmsnorm(h, ln_mix[l]))
        h = h + 0.5 * swiglu(rmsnorm(h, ln_ffn2[l]), ffn2_wgu[l], ffn2_wd[l])
        return h

    h = x
    for l in range(N_A_LAYERS):
        h = macaron_layer(h, l, lambda xn, l=l: moba_attention(xn, moba_wqkv[l], moba_wo[l], slopes))
    k_sh, v_sh = mla_shared_kv(h, kv_norm, mla_wdkv, ckv_norm, mla_wukv, cos, sin)
    for j in range(N_B_LAYERS):
        l = N_A_LAYERS + j
        h = macaron_layer(h, l, lambda xn, j=j: mla_attention(
            xn, mla_wdq[j], cq_norm[j], mla_wuq[j], mla_wo[j], k_sh, v_sh, cos, sin))
    return rmsnorm(h, final_norm)
```

```python
from concourse.bass_utils import run_bass_kernel_spmd
import numpy as np
from contextlib import ExitStack
import concourse.bass as bass
import concourse.mybir as mybir

F32 = mybir.dt.float32
BF16 = mybir.dt.bfloat16
AF = mybir.ActivationFunctionType
ALU = mybir.AluOpType
AX = mybir.AxisListType

ENGS = ("pe", "act", "dve", "pool", "sp")


class T:
    __slots__ = ("name", "ap", "last_w", "readers", "dma_sem", "dma_cnt")

    def __init__(self, name, ap):
        self.name = name
        self.ap = ap
        self.last_w = None
        self.readers = []
        self.dma_sem = None
        self.dma_cnt = 0

    def __getitem__(self, idx):
        return self.ap[idx]


class Op:
    __slots__ = ("q", "fn", "reads", "writes", "is_dma", "dst", "dma_val",
                 "flag", "rank", "waits", "idx")

    def __init__(self, q, fn, reads, writes, is_dma=False, dst=None):
        self.q = q
        self.fn = fn
        self.reads = reads
        self.writes = writes
        self.is_dma = is_dma
        self.dst = dst
        self.dma_val = 0
        self.flag = False
        self.rank = 0
        self.waits = []


class Prog:
    def __init__(self, nc):
        self.nc = nc
        self.ops = []
        self.es = ExitStack()
        self.tiles = []
        self.nsb = 0

    def sb(self, name, shape, dtype):
        h = self.es.enter_context(self.nc.sbuf_tensor(name, list(shape), dtype))
        t = T(name, h)
        self.tiles.append(t)
        return t

    def ps(self, name, shape, dtype=F32):
        h = self.es.enter_context(self.nc.psum_tensor(name, list(shape), dtype))
        t = T(name, h)
        self.tiles.append(t)
        return t

    def dram(self, name, shape, dtype, kind="Internal"):
        h = self.nc.dram_tensor(name, list(shape), dtype, kind=kind)
        t = T(name, h.ap())
        self.tiles.append(t)
        return t

    def view(self, name, ap):
        t = T(name, ap)
        self.tiles.append(t)
        return t

    def op(self, q, fn, reads=(), writes=()):
        o = Op(q, fn, list(reads), list(writes))
        self.ops.append(o)
        return o

    def dma(self, q, out_t, out_ap, in_ap, reads=(), extra_writes=(), **kw):
        def fn(eng, out_ap=out_ap, in_ap=in_ap, kw=kw):
            return eng.dma_start(out=out_ap, in_=in_ap, **kw)
        o = Op(q, fn, list(reads), [out_t] + list(extra_writes), is_dma=True, dst=out_t)
        self.ops.append(o)
        return o

    def analyze(self):
        ops = self.ops
        for i, o in enumerate(ops):
            o.idx = i
        deps_of = []
        for o in ops:
            deps = set()
            for t in o.reads:
                if t.last_w is not None:
                    deps.add(t.last_w)
            for t in o.writes:
                if t.last_w is not None:
                    deps.add(t.last_w)
                deps.update(t.readers)
            deps.discard(o.idx)
            real = []
            best = {}
            for d in deps:
                p = ops[d]
                if p.is_dma:
                    real.append(d)
                    continue
                if p.q == o.q and not o.is_dma:
                    if p.q == "pe":
                        continue
                    raw = any((t in p.writes) for t in o.reads)
                    if not raw:
                        continue
                if best.get(p.q, -1) < d:
                    best[p.q] = d
            real.extend(best.values())
            deps_of.append(real)
            for t in o.reads:
                t.readers.append(o.idx)
            for t in o.writes:
                t.last_w = o.idx
                t.readers = []
            if o.is_dma:
                o.dst.dma_cnt += 16
                o.dma_val = o.dst.dma_cnt
        for o, deps in zip(ops, deps_of):
            for d in deps:
                if not ops[d].is_dma:
                    ops[d].flag = True
        cnt = {e: 0 for e in ENGS}
        for o in ops:
            if o.flag and not o.is_dma:
                cnt[o.q] += 1
                o.rank = cnt[o.q]
        self.eng_cnt = cnt
        waited = {q: {} for q in ENGS}
        nw = 0
        for o, deps in zip(ops, deps_of):
            need = {}
            for d in deps:
                p = ops[d]
                if p.is_dma:
                    key = ("dma", id(p.dst))
                    val = p.dma_val
                    need[key] = max(need.get(key, 0), val)
                    need[(key, "t")] = p.dst
                else:
                    key = ("eng", p.q)
                    need[key] = max(need.get(key, 0), p.rank)
            w = waited[o.q]
            for key, val in list(need.items()):
                if isinstance(key[0], tuple):
                    continue
                if w.get(key, 0) >= val:
                    continue
                w[key] = val
                if key[0] == "dma":
                    o.waits.append(("dma", need[(key, "t")], val))
                else:
                    o.waits.append(("eng", key[1], val))
                nw += 1
        self.n_waits = nw

    def emit(self, final_waits=()):
        nc = self.nc
        self.analyze()
        es = self.es
        sems = {e: es.enter_context(nc.semaphore("s_" + e)) for e in ENGS if e != "sp"}
        nd = 0
        for t in self.tiles:
            if t.dma_cnt > 0:
                t.dma_sem = es.enter_context(nc.semaphore("d%d" % nd))
                nd += 1
        self.n_dma_sems = nd
        block = es.enter_context(nc.Block())
        by_q = {e: [o for o in self.ops if o.q == e] for e in ENGS}

        def run(eng, q):
            for o in by_q[q]:
                for w in o.waits:
                    if w[0] == "dma":
                        eng.wait_ge(w[1].dma_sem, w[2])
                    else:
                        eng.wait_ge(sems[w[1]], w[2])
                inst = o.fn(eng)
                if o.is_dma:
                    inst.then_inc(o.dst.dma_sem, 16)
                elif o.flag:
                    inst.then_inc(sems[o.q], 1)
            if q == "sp":
                for t in final_waits:
                    eng.wait_ge(t.dma_sem, t.dma_cnt)

        @block.tensor
        def _(e):
            run(e, "pe")

        @block.scalar
        def _(e):
            run(e, "act")

        @block.vector
        def _(e):
            run(e, "dve")

        @block.gpsimd
        def _(e):
            run(e, "pool")

        @block.sync
        def _(e):
            run(e, "sp")

    def close(self):
        self.es.close()


D = 2048
S = 8192
NCORE = 8
TL = 1024
NCH = 16
DFF = 5632
NFT = 44
EPS = 1e-6
BIG = 32768.0
HD = 128
NH = 16
QL, KVL, ROPE = 768, 512, 64


class Res:
    def __init__(self, P):
        self.P = P
        self.hT_raw = P.sb("hT", [128, NCH, TL], F32)
        self.hTc = [P.view("hT%d" % c, self.hT_raw[:, c, :]) for c in range(NCH)]
        self.pages = [P.sb("pg%d" % i, [128, 4096], BF16) for i in range(16)]
        self.banks = [P.ps("bk%d" % i, [128, 512], F32) for i in range(8)]
        self.ones = P.sb("ones", [128, 128], BF16)
        self.rstd = P.sb("rstd", [128, TL], F32)
        self.tmpf = [P.sb("tmpf%d" % i, [128, 512], F32) for i in range(3)]
        self.gains = P.sb("gains", [128, 96], F32)
        self.bi = 0
        P.op("dve", lambda e: e.memset(self.ones[:, :], 1.0), writes=[self.ones])
        self.epsc = P.sb("epsc", [128, 1], F32)
        P.op("dve", lambda e: e.memset(self.epsc[:, :], EPS), writes=[self.epsc])

    def bank(self):
        b = self.banks[self.bi % 8]
        self.bi += 1
        return b


def load_hT(P, R, h_dram):
    hv = h_dram.ap.rearrange("(c p) t -> p c t", p=128)
    for c in range(NCH):
        P.dma("sp", R.hTc[c], R.hTc[c].ap, hv[:, c, :], reads=[h_dram])


def store_hT(P, R, h_dram):
    hv = h_dram.ap.rearrange("(c p) t -> p c t", p=128)
    for c in range(NCH):
        P.dma("sp", h_dram, hv[:, c, :], R.hTc[c].ap, reads=[R.hTc[c]])


def load_gain(P, R, g_dram, col, n):
    P.dma("sp", R.gains, R.gains[:, col:col + n], g_dram.ap[:, :])


def rmsnorm_fm(P, R, srcs, gcol, outs, ntok=TL, final_inplace=False):
    n = len(srcs)
    dd = 128 * n
    for c, ((st, sap), (ot, oap)) in enumerate(zip(srcs, outs)):
        P.op("act", lambda e, sap=sap, oap=oap: e.activation(out=oap, in_=sap, func=AF.Square),
             reads=[st], writes=[ot])
    for t0 in range(0, ntok, 512):
        bk = R.bank()
        for c, (ot, oap) in enumerate(outs):
            P.op("pe", lambda e, bk=bk, oap=oap, c=c, t0=t0: e.matmul(
                bk[:, :], R.ones[:, :], oap[:, t0:t0 + 512], start=(c == 0), stop=(c == n - 1)),
                reads=[ot, R.ones], writes=[bk])
        P.op("act", lambda e, bk=bk, t0=t0: e.activation(
            out=R.rstd[:, t0:t0 + 512], in_=bk[:, :], func=AF.Sqrt, scale=1.0 / dd, bias=R.epsc[:, 0:1]),
            reads=[bk, R.epsc], writes=[R.rstd])
    P.op("dve", lambda e: e.reciprocal(out=R.rstd[:, :ntok], in_=R.rstd[:, :ntok]), reads=[R.rstd], writes=[R.rstd])
    for c, ((st, sap), (ot, oap)) in enumerate(zip(srcs, outs)):
        if final_inplace:
            ot, oap = st, sap
        P.op("dve", lambda e, sap=sap, oap=oap, c=c: e.scalar_tensor_tensor(
            out=oap, in0=sap, scalar=R.gains[:, gcol + c:gcol + c + 1], in1=R.rstd[:, :ntok],
            op0=ALU.mult, op1=ALU.mult), reads=[st, R.rstd, R.gains], writes=[ot])


def xn_views(R):
    v = []
    for c in range(NCH):
        pg = R.pages[c // 4]
        v.append((pg, pg[:, (c % 4) * 1024:(c % 4 + 1) * 1024]))
    return v


def norm_h(P, R, gcol):
    srcs = [(R.hTc[c], R.hTc[c].ap) for c in range(NCH)]
    outs = xn_views(R)
    rmsnorm_fm(P, R, srcs, gcol, outs)
    return outs


def ffn_step(P, R, gcol, wgu, wd):
    xn = norm_h(P, R, gcol)
    wguv = wgu.ap.rearrange("(c p) n -> p c n", p=128)
    wdv = wd.ap.rearrange("(g f p) n -> g p f n", p=128, f=4)
    act_pg = [R.pages[4], R.pages[5]]
    wgu_slots = [(R.pages[6 + 2 * i], R.pages[7 + 2 * i]) for i in range(3)]
    wd_slots = [(R.pages[12], R.pages[13]), (R.pages[14], R.pages[15])]

    def load_wgu(pi):
        gpg, upg = wgu_slots[pi % 3]
        P.dma("pool", gpg, gpg[:, :].rearrange("p (c n) -> p c n", c=16), wguv[:, :, pi * 256:(pi + 1) * 256])
        P.dma("pool", upg, upg[:, :].rearrange("p (c n) -> p c n", c=16),
              wguv[:, :, DFF + pi * 256:DFF + (pi + 1) * 256])

    def load_wd(g):
        a, b = wd_slots[g % 2]
        P.dma("pool", a, a[:, :].rearrange("p (f n) -> p f n", f=2), wdv[g, :, 0:2, :])
        P.dma("pool", b, b[:, :].rearrange("p (f n) -> p f n", f=2), wdv[g, :, 2:4, :])

    load_wgu(0)
    load_wgu(1)
    load_wd(0)
    ui = 0
    for g in range(11):
        apg = act_pg[g % 2]
        for ft in range(4):
            f = 4 * g + ft
            pi = f // 2
            if f % 2 == 0 and pi + 2 < 22:
                load_wgu(pi + 2)
            gpg, upg = wgu_slots[pi % 3]
            col = (f % 2) * 128
            for tt in range(2):
                gb, ub = R.bank(), R.bank()
                for (bk, wpg) in ((gb, gpg), (ub, upg)):
                    for c in range(NCH):
                        xt, xap = xn[c]
                        P.op("pe", lambda e, bk=bk, wpg=wpg, c=c, xap=xap, tt=tt, col=col: e.matmul(
                            bk[:, :], wpg[:, c * 256 + col:c * 256 + col + 128], xap[:, tt * 512:(tt + 1) * 512],
                            start=(c == 0), stop=(c == NCH - 1)), reads=[wpg, xt], writes=[bk])
                tf = R.tmpf[ui % 3]
                ui += 1
                P.op("act", lambda e, tf=tf, gb=gb: e.activation(out=tf[:, :], in_=gb[:, :], func=AF.Silu),
                     reads=[gb], writes=[tf])
                P.op("dve", lambda e, tf=tf, ub=ub, apg=apg, ft=ft, tt=tt: e.tensor_tensor(
                    out=apg[:, ft * 1024 + tt * 512:ft * 1024 + (tt + 1) * 512], in0=tf[:, :], in1=ub[:, :],
                    op=ALU.mult), reads=[tf, ub], writes=[apg])
        if g + 1 < 11:
            load_wd(g + 1)
        wa, wb = wd_slots[g % 2]
        for dt in range(NCH):
            for tt in range(2):
                bk = R.bank()
                for fc in range(4):
                    wpg = wa if fc < 2 else wb
                    off = (fc % 2) * 2048 + dt * 128
                    P.op("pe", lambda e, bk=bk, wpg=wpg, off=off, apg=apg, fc=fc, tt=tt: e.matmul(
                        bk[:, :], wpg[:, off:off + 128], apg[:, fc * 1024 + tt * 512:fc * 1024 + (tt + 1) * 512],
                        start=(fc == 0), stop=(fc == 3)), reads=[wpg, apg], writes=[bk])
                P.op("dve", lambda e, bk=bk, dt=dt, tt=tt: e.scalar_tensor_tensor(
                    out=R.hTc[dt].ap[:, tt * 512:(tt + 1) * 512], in0=bk[:, :], scalar=0.5,
                    in1=R.hTc[dt].ap[:, tt * 512:(tt + 1) * 512], op0=ALU.mult, op1=ALU.add),
                    reads=[bk, R.hTc[dt]], writes=[R.hTc[dt]])


MOBA_MODE = 2
SKIP = ''
def bf16_split3(v):
    import ml_dtypes
    v = np.asarray(v, np.float64)
    hi = v.astype(ml_dtypes.bfloat16).astype(np.float64)
    mid = (v - hi).astype(ml_dtypes.bfloat16).astype(np.float64)
    lo = (v - hi - mid).astype(ml_dtypes.bfloat16).astype(np.float64)
    return np.stack([hi, mid, lo]).astype(np.float32)


def attn_consts_host(core, moba):
    c = {}
    c["identF"] = np.eye(128, dtype=np.float32)
    kl = np.arange(128)[:, None]
    ql = np.arange(512)[None, :]
    mj = np.zeros((128, 4, 512), np.float32)
    for j in range(4):
        mj[:, j, :] = np.where(ql >= 128 * j + kl, 0.0, -BIG)
    c["MJ"] = mj.reshape(128, 2048)
    if moba:
        el = np.zeros((35, 32, 128), np.float32)
        for n in range(32):
            el[n, n, :] = 1.0
        el[32:35] = 1.0
        c["EL"] = el.reshape(35, 4096)
        a3 = np.zeros((2, 3, S), np.float32)
        ab = np.zeros((128, 2, 64), np.float32)
        for hl in range(2):
            h = 2 * core + hl
            m = 2.0 ** (-8.0 * (h + 1) / NH)
            a3[hl] = np.tile(bf16_split3(-m * np.arange(512) * (HD ** 0.5)), (1, 16))
            dd = np.arange(64) - 3
            ab[:, hl, :] = m * (np.arange(128)[:, None] - 128.0 * dd[None, :])
        c["A3"] = a3.reshape(6, S)
        c["AB"] = ab.reshape(128, 128)
        n = np.arange(32)[None, :]
        cur = np.arange(32)[:, None]
        pm = np.where(n < cur, 0.0, -1e30)
        c3 = np.where(n < cur, BIG, 0.0)
        c4 = np.where(n == cur, 0.0, -BIG)
        c["PM"] = np.broadcast_to(pm.reshape(1, 1024), (128, 1024)).astype(np.float32).copy()
        c["C3"] = np.broadcast_to(c3.reshape(1, 1024), (128, 1024)).astype(np.float32).copy()
        c["C4"] = np.broadcast_to(c4.reshape(1, 1024), (128, 1024)).astype(np.float32).copy()
    else:
        inv = 1.0 / (10000.0 ** (np.arange(0, ROPE, 2, dtype=np.float32) / ROPE))
        ang = np.arange(S, dtype=np.float32)[:, None] * inv[None, :]
        cos, sin = np.cos(ang).T, np.sin(ang).T
        c["C2"] = np.concatenate([cos, cos], 0).astype(np.float32)
        c["S2"] = np.concatenate([-sin, sin], 0).astype(np.float32)
    return c


class AttnRes:
    def __init__(self, P, moba):
        self.P = P
        self.pages = [P.sb("pg%d" % i, [128, 4096], BF16) for i in range(16)]
        self.banks = [P.ps("bk%d" % i, [128, 512], F32) for i in range(8)]
        self.bi = 0
        self.ones = P.sb("ones", [128, 128], BF16)
        P.op("dve", lambda e: e.memset(self.ones[:, :], 1.0), writes=[self.ones])
        self.identF = P.sb("identF", [128, 128], F32)
        self.identB = P.sb("identB", [128, 128], BF16)
        self.MJ = P.sb("MJ", [128, 2048], BF16)
        self.tmpf = [P.sb("tmpf%d" % i, [128, 512], F32) for i in range(3)]
        self.pt = [P.sb("pt%d" % i, [128, 512], BF16) for i in range(4)]
        self.ob = [P.sb("ob%d" % i, [128, 512], BF16) for i in range(2)]
        self.ti = 0
        if moba:
            self.EL = P.sb("EL", [35, 4096], BF16)
            self.AB = P.sb("AB", [128, 128], F32)
            self.PM = P.sb("PM", [128, 1024], F32)
            self.C3 = P.sb("C3", [128, 1024], F32)
            self.C4 = P.sb("C4", [128, 1024], F32)
            self.kmean = P.sb("kmean", [128, 32], F32)
            P.op("dve", lambda e: e.memset(self.kmean[:, :], 0.0), writes=[self.kmean])
            self.gs = [[P.sb("gs%d_%d" % (i, j), [128, 32], F32) for j in range(4)] for i in range(2)]
            self.top8 = [P.sb("top8_%d" % i, [128, 8], F32) for i in range(2)]
        else:
            self.KR = P.sb("KR", [64, S], BF16)
            self.rt = [[P.sb("rt%d_%d" % (i, j), [64, 512], F32) for j in range(2)] for i in range(2)]

    def bank(self):
        b = self.banks[self.bi % 4]
        self.bi += 1
        return b

    def tmp(self):
        t = self.tmpf[self.ti % 3]
        self.ti += 1
        return t


def attn_load_consts(P, A, cd, moba):
    P.dma("sp", A.identF, A.identF[:, :], cd["identF"].ap[:, :])
    P.dma("pool", A.identB, A.identB[:, :], cd["identF"].ap[:, :])
    P.dma("pool", A.MJ, A.MJ[:, :], cd["MJ"].ap[:, :])
    if moba:
        P.dma("pool", A.EL, A.EL[:, :], cd["EL"].ap[:, :])
        for nm in ("AB", "PM", "C3", "C4"):
            t = getattr(A, nm)
            P.dma("sp", t, t[:, :], cd[nm].ap[:, :])


def attn_tile_loop(P, A, QTi, qt_pages, kt_pages, v_pages, second, exp_bias, scale, ot_dram, orow, accs):
    nk = 4 * QTi + 4
    acc_o, acc_d = accs
    qpg = qt_pages[QTi // 8]
    qap = qpg[:, (QTi % 8) * 512:(QTi % 8 + 1) * 512]
    pend = None

    def pv(kt, pt):
        vpg = v_pages[kt // 32]
        P.op("pe", lambda e, vpg=vpg, kt=kt, pt=pt: e.matmul(
            acc_o[:, :], vpg[:, (kt % 32) * 128:(kt % 32 + 1) * 128], pt[:, :], start=(kt == 0), stop=(kt == nk - 1)),
            reads=[vpg, pt], writes=[acc_o])
        P.op("pe", lambda e, kt=kt, pt=pt: e.matmul(
            acc_d[:, :], A.ones[:, :], pt[:, :], start=(kt == 0), stop=(kt == nk - 1)),
            reads=[A.ones, pt], writes=[acc_d])

    for kt in range(nk):
        sbk = A.bank()
        kpg = kt_pages[kt // 32]
        diag = kt >= 4 * QTi
        P.op("pe", lambda e, sbk=sbk, kpg=kpg, kt=kt: e.matmul(
            sbk[:, :], kpg[:, (kt % 32) * 128:(kt % 32 + 1) * 128], qap, start=True, stop=False),
            reads=[kpg, qpg], writes=[sbk])
        second(sbk, kt, QTi, stop=not diag)
        if diag:
            j = kt - 4 * QTi
            P.op("pe", lambda e, sbk=sbk, j=j: e.matmul(
                sbk[:, :], A.identB[:, :], A.MJ[:, j * 512:(j + 1) * 512], start=False, stop=True),
                reads=[A.identB, A.MJ], writes=[sbk])
        pt = A.pt[kt % 4]
        bt, bap = exp_bias(kt, QTi)
        if bap is None:
            P.op("act", lambda e, pt=pt, sbk=sbk: e.activation(out=pt[:, :], in_=sbk[:, :], func=AF.Exp, scale=scale),
                 reads=[sbk], writes=[pt])
        else:
            P.op("act", lambda e, pt=pt, sbk=sbk, bap=bap: e.activation(
                out=pt[:, :], in_=sbk[:, :], func=AF.Exp, scale=scale, bias=bap), reads=[sbk, bt], writes=[pt])
        if pend is not None:
            pv(*pend)
        pend = (kt, pt)
    pv(*pend)
    rd = A.tmp()
    ob = A.ob[QTi % 2]
    P.op("dve", lambda e, rd=rd: e.reciprocal(out=rd[:, :], in_=acc_d[:, :]), reads=[acc_d], writes=[rd])
    P.op("dve", lambda e, rd=rd, ob=ob: e.tensor_tensor(out=ob[:, :], in0=acc_o[:, :], in1=rd[:, :], op=ALU.mult),
         reads=[acc_o, rd], writes=[ob])
    P.dma("sp", ot_dram, ot_dram.ap[orow:orow + 128, QTi * 512:(QTi + 1) * 512], ob[:, :], reads=[ob])


def moba_attn_phase(P, A, xg, wqkv, cd, ot_dram):
    attn_load_consts(P, A, cd, True)
    pg = A.pages
    qt_pages, kt_pages, v_pages, bt_pages = pg[0:2], pg[2:4], pg[4:6], pg[6:8]
    xslots = [(pg[8], pg[9]), (pg[10], pg[11])]
    wv_ = wqkv.ap.rearrange("(c p) n -> p c n", p=128)
    scale = HD ** -0.5
    a3 = cd["A3"]
    for hl in range(2):
        wpgs = (pg[12 + 2 * hl], pg[13 + 2 * hl])
        wq_pg, wk_pg, wv_pg = wpgs[0], wpgs[0], wpgs[1]
        P.dma("pool", wq_pg, wq_pg[:, 0:2048].rearrange("p (c n) -> p c n", c=16), wv_[:, :, hl * 128:(hl + 1) * 128])
        P.dma("pool", wk_pg, wk_pg[:, 2048:4096].rearrange("p (c n) -> p c n", c=16),
              wv_[:, :, 256 + hl * 128:256 + (hl + 1) * 128])
        P.dma("pool", wv_pg, wv_pg[:, 0:2048].rearrange("p (c n) -> p c n", c=16),
              wv_[:, :, 512 + hl * 128:512 + (hl + 1) * 128])
        for half in range(2):
            bpg = bt_pages[half]
            P.dma("pool", bpg, bpg[32:35, :], a3.ap[hl * 3:hl * 3 + 3, half * 4096:(half + 1) * 4096])

        def second(sbk, kt, QTi, stop):
            n = kt // 2
            bpg = bt_pages[QTi // 8]
            P.op("pe", lambda e, sbk=sbk, n=n, bpg=bpg, QTi=QTi, stop=stop: e.matmul(
                sbk[:, :], A.EL[0:35, n * 128:(n + 1) * 128], bpg[0:35, (QTi % 8) * 512:(QTi % 8 + 1) * 512],
                start=False, stop=stop), reads=[A.EL, bpg], writes=[sbk])

        def exp_bias(kt, QTi, hl=hl):
            dd = 4 * QTi - kt
            return A.AB, A.AB[:, hl * 64 + dd + 3:hl * 64 + dd + 4]

        for tt in range(16 if MOBA_MODE >= 0 else 0):
            r, half = tt // 2, tt % 2
            xa, xb = xslots[tt % 2]
            xv = xg.ap[r].rearrange("(c p) t -> p c t", p=128)
            P.dma("sp", xa, xa[:, :].rearrange("p (c t) -> p c t", c=8), xv[:, 0:8, half * 512:(half + 1) * 512])
            P.dma("sp", xb, xb[:, :].rearrange("p (c t) -> p c t", c=8), xv[:, 8:16, half * 512:(half + 1) * 512])

            def xc(c):
                xp = xa if c < 8 else xb
                return xp, xp[:, (c % 8) * 512:(c % 8 + 1) * 512]
            if 'K' in SKIP: continue
            kb = A.bank()
            for c in range(NCH):
                xp, xap = xc(c)
                P.op("pe", lambda e, kb=kb, c=c, xap=xap, wk_pg=wk_pg: e.matmul(
                    kb[:, :], wk_pg[:, 2048 + c * 128:2048 + (c + 1) * 128], xap, start=(c == 0), stop=(c == 15)),
                    reads=[wk_pg, xp], writes=[kb])
            kpg = kt_pages[tt // 8]
            P.op("act", lambda e, kb=kb, kpg=kpg, tt=tt: e.activation(
                out=kpg[:, (tt % 8) * 512:(tt % 8 + 1) * 512], in_=kb[:, :], func=AF.Copy), reads=[kb], writes=[kpg])
            P.op("dve", lambda e, kpg=kpg, tt=tt: e.tensor_reduce(
                out=A.kmean[:, 2 * tt:2 * tt + 2],
                in_=kpg[:, (tt % 8) * 512:(tt % 8 + 1) * 512].rearrange("p (b t) -> p b t", b=2),
                axis=AX.X, op=ALU.add), reads=[kpg], writes=[A.kmean])
            if 'Q' in SKIP: continue
            qb = A.bank()
            for c in range(NCH):
                xp, xap = xc(c)
                P.op("pe", lambda e, qb=qb, c=c, xap=xap, wq_pg=wq_pg: e.matmul(
                    qb[:, :], wq_pg[:, c * 128:(c + 1) * 128], xap, start=(c == 0), stop=(c == 15)),
                    reads=[wq_pg, xp], writes=[qb])
            qpg = qt_pages[tt // 8]
            qf = A.tmp()
            P.op("dve", lambda e, qb=qb, qf=qf: e.tensor_copy(out=qf[:, :], in_=qb[:, :]), reads=[qb], writes=[qf])
            P.op("act", lambda e, qf=qf, qpg=qpg, tt=tt: e.activation(
                out=qpg[:, (tt % 8) * 512:(tt % 8 + 1) * 512], in_=qf[:, :], func=AF.Copy), reads=[qf], writes=[qpg])
            if 'V' in SKIP: continue
            vb = A.bank()
            for sub in range(4):
                for c in range(NCH):
                    xp, xap = xc(c)
                    P.op("pe", lambda e, vb=vb, c=c, xap=xap, sub=sub, wv_pg=wv_pg: e.matmul(
                        vb[:, sub * 128:(sub + 1) * 128], xap[:, sub * 128:(sub + 1) * 128],
                        wv_pg[:, c * 128:(c + 1) * 128], start=(c == 0), stop=(c == 15)),
                        reads=[wv_pg, xp], writes=[vb])
            vpg = v_pages[tt // 8]
            P.op("dve", lambda e, vb=vb, vpg=vpg, tt=tt: e.tensor_copy(
                out=vpg[:, (tt % 8) * 512:(tt % 8 + 1) * 512], in_=vb[:, :]), reads=[vb], writes=[vpg])
            for sub in (range(4) if MOBA_MODE >= 1 else []):
                i128 = 4 * tt + sub
                cur = i128 // 2
                g0, g1, g2, g3 = A.gs[i128 % 2]
                t8 = A.top8[i128 % 2]
                gb = A.bank()
                P.op("pe", lambda e, gb=gb, qf=qf, sub=sub: e.matmul(
                    gb[:, 0:32], qf[:, sub * 128:(sub + 1) * 128], A.kmean[:, :], start=True, stop=True),
                    reads=[qf, A.kmean], writes=[gb])
                P.op("dve", lambda e, gb=gb, g0=g0, cur=cur: e.tensor_tensor(
                    out=g0[:, :], in0=gb[:, 0:32], in1=A.PM[:, cur * 32:(cur + 1) * 32], op=ALU.add),
                    reads=[gb, A.PM], writes=[g0])
                P.op("dve", lambda e, g0=g0, t8=t8: e.max(out=t8[:, :], in_=g0[:, :]), reads=[g0], writes=[t8])
                P.op("dve", lambda e, g0=g0, g1=g1, t8=t8: e.tensor_scalar(
                    out=g1[:, :], in0=g0[:, :], scalar1=t8[:, 2:3], scalar2=BIG, op0=ALU.is_ge, op1=ALU.mult),
                    reads=[g0, t8], writes=[g1])
                P.op("dve", lambda e, g1=g1, g2=g2, cur=cur: e.tensor_tensor(
                    out=g2[:, :], in0=g1[:, :], in1=A.C3[:, cur * 32:(cur + 1) * 32], op=ALU.min),
                    reads=[g1, A.C3], writes=[g2])
                P.op("dve", lambda e, g2=g2, g3=g3, cur=cur: e.tensor_tensor(
                    out=g3[:, :], in0=g2[:, :], in1=A.C4[:, cur * 32:(cur + 1) * 32], op=ALU.add),
                    reads=[g2, A.C4], writes=[g3])
                tb = A.bank()
                P.op("pe", lambda e, tb=tb, g3=g3: e.transpose(tb[0:32, 0:128], g3[:, :], A.identF[:, :]),
                     reads=[g3, A.identF], writes=[tb])
                bpg = bt_pages[i128 // 32]
                P.op("act", lambda e, tb=tb, bpg=bpg, i128=i128: e.activation(
                    out=bpg[0:32, (i128 % 32) * 128:(i128 % 32 + 1) * 128], in_=tb[0:32, 0:128], func=AF.Copy),
                    reads=[tb], writes=[bpg])
            accs = (A.banks[4 + 2 * (tt % 2)], A.banks[5 + 2 * (tt % 2)])
            if MOBA_MODE >= 2:
              attn_tile_loop(P, A, tt, qt_pages, kt_pages, v_pages, second, exp_bias, scale, ot_dram, hl * 128, accs)


def f32view(pg):
    return pg[:, :].bitcast(F32)


def store_pages_fm(P, pages_views, dram, nchunk):
    dv = dram.ap.rearrange("(c p) t -> p c t", p=128)
    for c in range(nchunk):
        t, ap = pages_views[c]
        P.dma("sp", dram, dv[:, c, :], ap, reads=[t])


def oproj_step(P, R, o_dram, wo):
    ov = xn_views(R)
    odv = o_dram.ap.rearrange("(c p) t -> p c t", p=128)
    for c in range(NCH):
        t, ap = ov[c]
        P.dma("sp", t, ap, odv[:, c, :])
    wv = wo.ap.rearrange("(c p) n -> p c n", p=128)
    slots = [R.pages[6 + i] for i in range(6)]
    for pi in range(8):
        spg = slots[pi % 6]
        P.dma("pool", spg, spg[:, :].rearrange("p (c n) -> p c n", c=16), wv[:, :, pi * 256:(pi + 1) * 256])
        for k in range(2):
            dt = 2 * pi + k
            for tt in range(2):
                bk = R.bank()
                for c in range(NCH):
                    t, ap = ov[c]
                    P.op("pe", lambda e, bk=bk, spg=spg, c=c, k=k, ap=ap, tt=tt: e.matmul(
                        bk[:, :], spg[:, c * 256 + k * 128:c * 256 + (k + 1) * 128], ap[:, tt * 512:(tt + 1) * 512],
                        start=(c == 0), stop=(c == NCH - 1)), reads=[spg, t], writes=[bk])
                P.op("dve", lambda e, bk=bk, dt=dt, tt=tt: e.tensor_tensor(
                    out=R.hTc[dt].ap[:, tt * 512:(tt + 1) * 512], in0=bk[:, :],
                    in1=R.hTc[dt].ap[:, tt * 512:(tt + 1) * 512], op=ALU.add),
                    reads=[bk, R.hTc[dt]], writes=[R.hTc[dt]])


def proj_fm(P, R, xn, w, ncols_tiles, col0, wslot_pages, evac):
    wv = w.ap.rearrange("(c p) n -> p c n", p=128)
    for ot, M in enumerate(ncols_tiles):
        spg = wslot_pages[ot % len(wslot_pages)]
        c0 = col0 + sum(ncols_tiles[:ot])
        P.dma("pool", spg, spg[:, 0:16 * M].rearrange("p (c n) -> p c n", c=16), wv[:, :, c0:c0 + M])
        for tt in range(2):
            bk = R.bank()
            for c in range(NCH):
                t, ap = xn[c]
                P.op("pe", lambda e, bk=bk, spg=spg, c=c, ap=ap, tt=tt, M=M: e.matmul(
                    bk[0:M, :], spg[:, c * M:(c + 1) * M], ap[:, tt * 512:(tt + 1) * 512],
                    start=(c == 0), stop=(c == NCH - 1)), reads=[spg, t], writes=[bk])
            evac(ot, tt, bk, M)


def qpre_step(P, R, gcol_mix, wdq, gcol_cq, cq_out):
    xn = norm_h(P, R, gcol_mix)
    cqf = [(R.pages[4 + c // 2], f32view(R.pages[4 + c // 2])[:, (c % 2) * 1024:(c % 2 + 1) * 1024]) for c in range(6)]

    def evac(ot, tt, bk, M):
        t, ap = cqf[ot]
        P.op("act", lambda e, bk=bk, ap=ap, tt=tt: e.activation(out=ap[:, tt * 512:(tt + 1) * 512], in_=bk[:, :],
                                                               func=AF.Copy), reads=[bk], writes=[t])
    proj_fm(P, R, xn, wdq, [128] * 6, 0, [R.pages[9], R.pages[10], R.pages[11]], evac)
    outs = [(R.pages[7 + c // 4], R.pages[7 + c // 4][:, (c % 4) * 1024:(c % 4 + 1) * 1024]) for c in range(6)]
    rmsnorm_fm(P, R, cqf, gcol_cq, outs)
    store_pages_fm(P, outs, cq_out, 6)


def kvpre_step(P, R, gcol_kv, wdkv_ext, gcol_ckv, c2, s2, ckv_out, kr_out):
    xn = norm_h(P, R, gcol_kv)
    ckf = [(R.pages[4 + c // 2], f32view(R.pages[4 + c // 2])[:, (c % 2) * 1024:(c % 2 + 1) * 1024]) for c in range(4)]
    tabs = f32view(R.pages[6])
    P.dma("sp", R.pages[6], tabs[0:64, 0:1024], c2.ap[:, :])
    P.dma("sp", R.pages[6], tabs[0:64, 1024:2048], s2.ap[:, :])
    rf = f32view(R.pages[12])
    krb = R.pages[13]

    def evac(ot, tt, bk, M):
        if ot < 4:
            t, ap = ckf[ot]
            P.op("act", lambda e, bk=bk, ap=ap, tt=tt: e.activation(out=ap[:, tt * 512:(tt + 1) * 512], in_=bk[:, :],
                                                                   func=AF.Copy), reads=[bk], writes=[t])
        else:
            off = 0 if ot == 4 else 1024
            P.op("dve", lambda e, bk=bk, tt=tt, off=off: e.tensor_tensor(
                out=rf[0:64, off + tt * 512:off + (tt + 1) * 512], in0=bk[0:64, :],
                in1=tabs[0:64, off + tt * 512:off + (tt + 1) * 512], op=ALU.mult),
                reads=[bk, R.pages[6]], writes=[R.pages[12]])
    proj_fm(P, R, xn, wdkv_ext, [128] * 4 + [64, 64], 0, [R.pages[9], R.pages[10], R.pages[11]], evac)
    P.op("dve", lambda e: e.tensor_tensor(out=krb[0:64, 0:1024], in0=rf[0:64, 0:1024], in1=rf[0:64, 1024:2048],
                                          op=ALU.add), reads=[R.pages[12]], writes=[krb])
    P.dma("sp", kr_out, kr_out.ap[:, :], krb[0:64, 0:1024], reads=[krb])
    outs = [(R.pages[7], R.pages[7][:, c * 1024:(c + 1) * 1024]) for c in range(4)]
    rmsnorm_fm(P, R, ckf, gcol_ckv, outs)
    store_pages_fm(P, outs, ckv_out, 4)


def final_norm_step(P, R, gcol):
    srcs = [(R.hTc[c], R.hTc[c].ap) for c in range(NCH)]
    outs = xn_views(R)
    rmsnorm_fm(P, R, srcs, gcol, outs, final_inplace=True)


def mla_attn_phase(P, A, cqg, ckvg, krg, wuq, wukv, cd, ot_dram):
    attn_load_consts(P, A, cd, False)
    pg = A.pages
    qt_pages, kt_pages, v_pages, qr_pages = pg[0:2], pg[2:4], pg[4:6], pg[6:8]
    cq_slots, ckv_slots = [pg[8], pg[9]], [pg[10], pg[11]]
    scale = (HD + ROPE) ** -0.5
    for r in range(8):
        P.dma("sp", A.KR, A.KR[:, r * TL:(r + 1) * TL], krg.ap[r])
    wuqv = wuq.ap.rearrange("(c p) n -> p c n", p=128)
    wukvv = wukv.ap.rearrange("(c p) n -> p c n", p=128)
    for hl in range(2):
        wpg = pg[12 + hl]
        P.dma("pool", wpg, wpg[:, 0:1536].rearrange("p (c n) -> p c n", c=6), wuqv[:, :, hl * 256:(hl + 1) * 256])
        P.dma("pool", wpg, wpg[:, 2048:3072].rearrange("p (c n) -> p c n", c=4), wukvv[:, :, hl * 256:(hl + 1) * 256])

        def second(sbk, kt, QTi, stop):
            qrp = qr_pages[QTi // 8]
            P.op("pe", lambda e, sbk=sbk, kt=kt, qrp=qrp, QTi=QTi, stop=stop: e.matmul(
                sbk[:, :], A.KR[0:64, kt * 128:(kt + 1) * 128], qrp[0:64, (QTi % 8) * 512:(QTi % 8 + 1) * 512],
                start=False, stop=stop), reads=[A.KR, qrp], writes=[sbk])

        def exp_bias(kt, QTi):
            return None, None

        for tt in range(16):
            r, half = tt // 2, tt % 2
            cqs, cks = cq_slots[tt % 2], ckv_slots[tt % 2]
            P.dma("sp", cqs, cqs[:, 0:3072].rearrange("p (c t) -> p c t", c=6),
                  cqg.ap[r].rearrange("(c p) t -> p c t", p=128)[:, :, half * 512:(half + 1) * 512])
            P.dma("sp", cks, cks[:, 0:2048].rearrange("p (c t) -> p c t", c=4),
                  ckvg.ap[r].rearrange("(c p) t -> p c t", p=128)[:, :, half * 512:(half + 1) * 512])
            ct, st = A.rt[tt % 2]
            P.dma("sp", ct, ct[:, :], cd["C2"].ap[:, tt * 512:(tt + 1) * 512])
            P.dma("sp", st, st[:, :], cd["S2"].ap[:, tt * 512:(tt + 1) * 512])
            kb = A.bank()
            for c in range(4):
                P.op("pe", lambda e, kb=kb, c=c, wpg=wpg, cks=cks: e.matmul(
                    kb[:, :], wpg[:, 2048 + c * 256:2048 + c * 256 + 128], cks[:, c * 512:(c + 1) * 512],
                    start=(c == 0), stop=(c == 3)), reads=[wpg, cks], writes=[kb])
            kpg = kt_pages[tt // 8]
            P.op("act", lambda e, kb=kb, kpg=kpg, tt=tt: e.activation(
                out=kpg[:, (tt % 8) * 512:(tt % 8 + 1) * 512], in_=kb[:, :], func=AF.Copy), reads=[kb], writes=[kpg])
            vb = A.bank()
            for sub in range(4):
                for c in range(4):
                    P.op("pe", lambda e, vb=vb, c=c, sub=sub, wpg=wpg, cks=cks: e.matmul(
                        vb[:, sub * 128:(sub + 1) * 128], cks[:, c * 512 + sub * 128:c * 512 + (sub + 1) * 128],
                        wpg[:, 2048 + c * 256 + 128:2048 + (c + 1) * 256], start=(c == 0), stop=(c == 3)),
                        reads=[wpg, cks], writes=[vb])
            vpg = v_pages[tt // 8]
            P.op("dve", lambda e, vb=vb, vpg=vpg, tt=tt: e.tensor_copy(
                out=vpg[:, (tt % 8) * 512:(tt % 8 + 1) * 512], in_=vb[:, :]), reads=[vb], writes=[vpg])
            qb = A.bank()
            for c in range(6):
                P.op("pe", lambda e, qb=qb, c=c, wpg=wpg, cqs=cqs: e.matmul(
                    qb[:, :], wpg[:, c * 256:c * 256 + 128], cqs[:, c * 512:(c + 1) * 512],
                    start=(c == 0), stop=(c == 5)), reads=[wpg, cqs], writes=[qb])
            qpg = qt_pages[tt // 8]
            P.op("act", lambda e, qb=qb, qpg=qpg, tt=tt: e.activation(
                out=qpg[:, (tt % 8) * 512:(tt % 8 + 1) * 512], in_=qb[:, :], func=AF.Copy), reads=[qb], writes=[qpg])
            rb, sb_ = A.bank(), A.bank()
            for (bk, o) in ((rb, 128), (sb_, 192)):
                for c in range(6):
                    P.op("pe", lambda e, bk=bk, c=c, o=o, wpg=wpg, cqs=cqs: e.matmul(
                        bk[0:64, :], wpg[:, c * 256 + o:c * 256 + o + 64], cqs[:, c * 512:(c + 1) * 512],
                        start=(c == 0), stop=(c == 5)), reads=[wpg, cqs], writes=[bk])
            t1, t2 = A.tmp(), A.tmp()
            P.op("dve", lambda e, rb=rb, t1=t1, ct=ct: e.tensor_tensor(out=t1[0:64, :], in0=rb[0:64, :], in1=ct[:, :],
                                                                      op=ALU.mult), reads=[rb, ct], writes=[t1])
            P.op("dve", lambda e, sb_=sb_, t2=t2, st=st: e.tensor_tensor(out=t2[0:64, :], in0=sb_[0:64, :], in1=st[:, :],
                                                                        op=ALU.mult), reads=[sb_, st], writes=[t2])
            qrp = qr_pages[tt // 8]
            P.op("dve", lambda e, t1=t1, t2=t2, qrp=qrp, tt=tt: e.tensor_tensor(
                out=qrp[0:64, (tt % 8) * 512:(tt % 8 + 1) * 512], in0=t1[0:64, :], in1=t2[0:64, :], op=ALU.add),
                reads=[t1, t2], writes=[qrp])
            accs = (A.banks[4 + 2 * (tt % 2)], A.banks[5 + 2 * (tt % 2)])
            attn_tile_loop(P, A, tt, qt_pages, kt_pages, v_pages, second, exp_bias, scale, ot_dram, hl * 128, accs)


def glay(g):
    g = np.asarray(g, np.float32)
    return np.ascontiguousarray(g.reshape(-1, 128).T)


_PROGS = {}


def build_token_prog(oproj, ffnA, kvpre, ffnB, pre):
    key = ("tok", oproj, ffnA, kvpre, ffnB, pre)
    if key in _PROGS:
        return _PROGS[key]
    nc = bass.Bass("TRN2", target_bir_lowering=False)
    P = Prog(nc)
    R = Res(P)
    di = lambda n, s, dt=F32: P.dram(n, s, dt, kind="ExternalInput")
    do = lambda n, s, dt=F32: P.dram(n, s, dt, kind="ExternalOutput")
    h_in = di("h_in", [D, TL])
    outs = []
    load_hT(P, R, h_in)
    gc = 0
    if oproj:
        o_in, wo = di("o_in", [D, TL], BF16), di("wo", [D, D])
        oproj_step(P, R, o_in, wo)
    if ffnA:
        g, wgu, wd = di("gA", [128, NCH]), di("wguA", [D, 2 * DFF]), di("wdA", [DFF, D])
        load_gain(P, R, g, gc, NCH)
        ffn_step(P, R, gc, wgu, wd)
        gc += NCH
    if pre != "final":
        h_out = do("h_out", [D, TL])
        outs.append(h_out)
    if kvpre:
        g1, g2 = di("g_kv", [128, NCH]), di("g_ckv", [128, 4])
        wdkv, c2, s2 = di("wdkv", [D, 640]), di("c2", [64, TL]), di("s2", [64, TL])
        ckv_out, kr_out = do("ckv_out", [KVL, TL], BF16), do("kr_out", [ROPE, TL], BF16)
        outs += [ckv_out, kr_out]
        load_gain(P, R, g1, 48, NCH)
        load_gain(P, R, g2, 64, 4)
        kvpre_step(P, R, 48, wdkv, 64, c2, s2, ckv_out, kr_out)
    if ffnB:
        g, wgu, wd = di("gB", [128, NCH]), di("wguB", [D, 2 * DFF]), di("wdB", [DFF, D])
        load_gain(P, R, g, gc, NCH)
        ffn_step(P, R, gc, wgu, wd)
        gc += NCH
    if pre != "final":
        store_hT(P, R, h_out)
    gp = di("g_pre", [128, NCH])
    load_gain(P, R, gp, gc, NCH)
    if pre == "moba":
        xn_out = do("xn_out", [D, TL], BF16)
        outs.append(xn_out)
        xn = norm_h(P, R, gc)
        store_pages_fm(P, xn, xn_out, NCH)
    elif pre == "mla":
        wdq, gq = di("wdq", [D, QL]), di("g_cq", [128, 6])
        cq_out = do("cq_out", [QL, TL], BF16)
        outs.append(cq_out)
        load_gain(P, R, gq, 68, 6)
        qpre_step(P, R, gc, wdq, 68, cq_out)
    else:
        y = do("y_out", [D, TL])
        outs.append(y)
        final_norm_step(P, R, gc)
        store_hT(P, R, y)
    P.emit(final_waits=outs)
    P.close()
    _PROGS[key] = nc
    return nc


MOBA_CN = ["identF", "MJ", "EL", "A3", "AB", "PM", "C3", "C4"]
MLA_CN = ["identF", "MJ", "C2", "S2"]


def build_attn_prog(moba):
    key = ("attn", moba)
    if key in _PROGS:
        return _PROGS[key]
    nc = bass.Bass("TRN2", target_bir_lowering=False)
    P = Prog(nc)
    ch = attn_consts_host(0, moba)
    names = MOBA_CN if moba else MLA_CN
    cd = {k: P.dram("c_" + k, list(ch[k].shape), F32, kind="ExternalInput") for k in names}
    ot = P.dram("ot", [256, S], BF16, kind="ExternalOutput")
    A = AttnRes(P, moba)
    if moba:
        xg = P.dram("xg", [8, D, TL], BF16, kind="ExternalInput")
        wqkv = P.dram("wqkv", [D, 768], F32, kind="ExternalInput")
        moba_attn_phase(P, A, xg, wqkv, cd, ot)
    else:
        cqg = P.dram("cqg", [8, QL, TL], BF16, kind="ExternalInput")
        ckvg = P.dram("ckvg", [8, KVL, TL], BF16, kind="ExternalInput")
        krg = P.dram("krg", [8, ROPE, TL], BF16, kind="ExternalInput")
        wuq = P.dram("wuq", [QL, 512], F32, kind="ExternalInput")
        wukv = P.dram("wukv", [KVL, 512], F32, kind="ExternalInput")
        mla_attn_phase(P, A, cqg, ckvg, krg, wuq, wukv, cd, ot)
    P.emit(final_waits=[ot])
    P.close()
    _PROGS[key] = nc
    return nc


def _run(nc, ims):
    res = run_bass_kernel_spmd(nc, ims, core_ids=list(range(NCORE)))
    return res.results


def kernel(x, ln_ffn1, ffn1_wgu, ffn1_wd, ln_mix, ln_ffn2, ffn2_wgu, ffn2_wd,
           moba_wqkv, moba_wo, kv_norm, mla_wdkv, ckv_norm, mla_wukv,
           mla_wdq, cq_norm, mla_wuq, mla_wo, final_norm):
    f = lambda a: np.asarray(a, np.float32)
    x = f(x)[0]
    hT = [np.ascontiguousarray(x[c * TL:(c + 1) * TL].T) for c in range(NCORE)]
    moba_c = [attn_consts_host(c, True) for c in range(NCORE)]
    mla_c = attn_consts_host(0, False)
    ffn1_wgu, ffn1_wd, ffn2_wgu, ffn2_wd = f(ffn1_wgu), f(ffn1_wd), f(ffn2_wgu), f(ffn2_wd)
    moba_wqkv, moba_wo, mla_wo, mla_wdq, mla_wuq = f(moba_wqkv), f(moba_wo), f(mla_wo), f(mla_wdq), f(mla_wuq)
    mla_wdkv, mla_wukv = f(mla_wdkv), f(mla_wukv)
    half = ROPE // 2
    wdkv_ext = np.ascontiguousarray(np.concatenate(
        [mla_wdkv, mla_wdkv[:, KVL + half:KVL + ROPE], mla_wdkv[:, KVL:KVL + half]], axis=1))
    o_full = None
    for l in range(4):
        moba = l < 2
        ims = []
        for c in range(NCORE):
            im = {"h_in": hT[c]}
            if l > 0:
                wo_prev = moba_wo[l - 1] if (l - 1) < 2 else mla_wo[l - 3]
                im.update(o_in=np.ascontiguousarray(o_full[:, c * TL:(c + 1) * TL]), wo=wo_prev,
                          gA=glay(ln_ffn2[l - 1]), wguA=ffn2_wgu[l - 1], wdA=ffn2_wd[l - 1])
            if l == 2:
                im.update(g_kv=glay(kv_norm), g_ckv=glay(ckv_norm), wdkv=wdkv_ext,
                          c2=np.ascontiguousarray(mla_c["C2"][:, c * TL:(c + 1) * TL]),
                          s2=np.ascontiguousarray(mla_c["S2"][:, c * TL:(c + 1) * TL]))
            im.update(gB=glay(ln_ffn1[l]), wguB=ffn1_wgu[l], wdB=ffn1_wd[l], g_pre=glay(ln_mix[l]))
            if not moba:
                im.update(wdq=mla_wdq[l - 2], g_cq=glay(cq_norm[l - 2]))
            ims.append(im)
        nc = build_token_prog(l > 0, l > 0, l == 2, True, "moba" if moba else "mla")
        res = _run(nc, ims)
        hT = [res[c]["h_out"] for c in range(NCORE)]
        if l == 2:
            ckvg = np.stack([res[c]["ckv_out"] for c in range(NCORE)])
            krg = np.stack([res[c]["kr_out"] for c in range(NCORE)])
        ims = []
        if moba:
            xg = np.stack([res[c]["xn_out"] for c in range(NCORE)])
            W = moba_wqkv[l]
            for c in range(NCORE):
                cols = [W[:, part * D + (2 * c + hl) * HD: part * D + (2 * c + hl + 1) * HD]
                        for part in range(3) for hl in range(2)]
                im = {"xg": xg, "wqkv": np.ascontiguousarray(np.concatenate(cols, 1))}
                for k in MOBA_CN:
                    im["c_" + k] = moba_c[c][k]
                ims.append(im)
        else:
            cqg = np.stack([res[c]["cq_out"] for c in range(NCORE)])
            Wq, Wkv = mla_wuq[l - 2], mla_wukv
            for c in range(NCORE):
                qc, kc = [], []
                for hl in range(2):
                    h = 2 * c + hl
                    b = h * (HD + ROPE)
                    qc += [Wq[:, b:b + HD], Wq[:, b + HD:b + HD + ROPE],
                           Wq[:, b + HD + half:b + HD + ROPE], Wq[:, b + HD:b + HD + half]]
                    kc += [Wkv[:, h * 256:h * 256 + 256]]
                im = {"cqg": cqg, "ckvg": ckvg, "krg": krg,
                      "wuq": np.ascontiguousarray(np.concatenate(qc, 1)),
                      "wukv": np.ascontiguousarray(np.concatenate(kc, 1))}
                for k in MLA_CN:
                    im["c_" + k] = mla_c[k]
                ims.append(im)
        res = _run(build_attn_prog(moba), ims)
        o_full = np.concatenate([res[c]["ot"] for c in range(NCORE)], axis=0)
    ims = []
    for c in range(NCORE):
        ims.append({"h_in": hT[c], "o_in": np.ascontiguousarray(o_full[:, c * TL:(c + 1) * TL]), "wo": mla_wo[1],
                    "gA": glay(ln_ffn2[3]), "wguA": ffn2_wgu[3], "wdA": ffn2_wd[3], "g_pre": glay(final_norm)})
    res = _run(build_token_prog(True, True, False, False, "final"), ims)
    out = np.concatenate([res[c]["y_out"].T for c in range(NCORE)], axis=0)
    return np.ascontiguousarray(out[None].astype(np.float32))
```
